# Optimizing a Trainium2 kernel written in Bass

```python
import jax, jax.numpy as jnp
from jax import lax
import numpy as np

D_MODEL = 4096
BATCH = 1
SEQ = 8192
DEPTH = 2

N_MIXERS = 2
POOL_WINDOWS = (2, 4, 8, 16)
N_POOL_GROUPS = len(POOL_WINDOWS)
POOL_GROUP_DIM = D_MODEL // N_POOL_GROUPS
HEAD_DIM = 128
N_HEADS = D_MODEL // HEAD_DIM
Q_BLOCK = 128
D_FF = 11008
N_EXPERTS = 8
TOP_K = 2
D_FF_EXPERT = 5632
LN_EPS = 1e-5
DEEPNORM_ALPHA = (2 * DEPTH) ** 0.25
DEEPNORM_BETA = (8 * DEPTH) ** -0.25

kernel_name = 'hybrid_pool_stickbreak_moe_deepnorm'


def layer_norm(x, g, b):
    xf = x.astype(jnp.float32)
    mu = jnp.mean(xf, axis=-1, keepdims=True)
    var = jnp.mean(jnp.square(xf - mu), axis=-1, keepdims=True)
    y = (xf - mu) * lax.rsqrt(var + LN_EPS)
    return (y * g.astype(jnp.float32) + b.astype(jnp.float32)).astype(x.dtype)


def pool_mixer(x, w_in, w_group, scale):
    B, S, D = x.shape
    u = (x @ w_in).astype(jnp.float32).reshape(B, S, N_POOL_GROUPS, POOL_GROUP_DIM)
    cs = jnp.cumsum(u, axis=1)
    cs_pad = jnp.concatenate([jnp.zeros((B, 1, N_POOL_GROUPS, POOL_GROUP_DIM), jnp.float32), cs], axis=1)
    pos = jnp.arange(1, S + 1)
    diffs = []
    for g, w in enumerate(POOL_WINDOWS):
        c = cs_pad[:, :, g]
        hi = c[:, 1:]
        lo = jnp.concatenate([jnp.zeros((B, w - 1, POOL_GROUP_DIM), jnp.float32), c[:, :S - w + 1]], axis=1)
        count = jnp.minimum(pos, w).astype(jnp.float32)[None, :, None]
        diffs.append((hi - lo) / count - u[:, :, g])
    d = jnp.stack(diffs, axis=2).astype(x.dtype)
    y = jnp.einsum('bsgc,gcd->bsgd', d, w_group).reshape(B, S, D)
    return y * scale


def stick_breaking_attention(x, w_qkv, w_o):
    B, S, D = x.shape
    qkv = (x @ w_qkv).reshape(B, S, 3, N_HEADS, HEAD_DIM)
    q = qkv[:, :, 0].astype(jnp.float32) * (HEAD_DIM ** -0.5)
    k = qkv[:, :, 1].astype(jnp.float32).transpose(0, 2, 1, 3)
    v = qkv[:, :, 2].astype(jnp.float32).transpose(0, 2, 1, 3)
    nb = S // Q_BLOCK
    q_blocks = q.reshape(B, nb, Q_BLOCK, N_HEADS, HEAD_DIM).transpose(1, 0, 3, 2, 4)
    starts = jnp.arange(nb, dtype=jnp.int32) * Q_BLOCK
    key_pos = jnp.arange(S, dtype=jnp.int32)

    def block(args):
        qi, t0 = args
        z = jnp.einsum('bhqd,bhkd->bhqk', qi, k)
        t = t0 + jnp.arange(Q_BLOCK, dtype=jnp.int32)
        mask = key_pos[None, :] < t[:, None]
        log_keep = jnp.where(mask, jax.nn.log_sigmoid(-z), 0.0)
        suffix = lax.cumsum(log_keep, axis=3, reverse=True) - log_keep
        a = jnp.where(mask, jnp.exp(jax.nn.log_sigmoid(z) + suffix), 0.0)
        return jnp.einsum('bhqk,bhkd->bhqd', a, v)

    o = lax.map(block, (q_blocks, starts))
    o = o.transpose(1, 0, 3, 2, 4).reshape(B, S, D).astype(x.dtype)
    return o @ w_o


def swiglu(h, w_gate, w_up, w_down):
    return (jax.nn.silu(h @ w_gate) * (h @ w_up)) @ w_down


def moe_swiglu(x, w_router, w_gate, w_up, w_down):
    B, S, D = x.shape
    h = x.reshape(B * S, D)
    logits = (h @ w_router).astype(jnp.float32)
    top_vals, top_idx = lax.top_k(logits, TOP_K)
    gates = jax.nn.softmax(top_vals, axis=-1)
    combine = jnp.sum(jax.nn.one_hot(top_idx, N_EXPERTS, dtype=jnp.float32) * gates[..., None], axis=1)
    y = jnp.zeros((B * S, D), jnp.float32)
    for e in range(N_EXPERTS):
        y = y + combine[:, e:e + 1] * swiglu(h, w_gate[e], w_up[e], w_down[e]).astype(jnp.float32)
    return y.reshape(B, S, D).astype(x.dtype)


def setup_inputs(seed: int = 0) -> dict:
    key = jax.random.key(seed)
    ks = jax.random.split(key, 22)
    d = D_MODEL

    def nrm(k, shape, scale):
        return jax.random.normal(k, shape, jnp.float32) * scale

    return {
        'x': nrm(ks[0], (BATCH, SEQ, d), 1.0),
        'l0_pool_w_in': nrm(ks[1], (d, d), d ** -0.5),
        'l0_pool_w_group': nrm(ks[2], (N_POOL_GROUPS, POOL_GROUP_DIM, POOL_GROUP_DIM), POOL_GROUP_DIM ** -0.5 * DEEPNORM_BETA),
        'l0_pool_scale': 1.0 + nrm(ks[3], (d,), 0.02),
        'l0_ln1_g': 1.0 + nrm(ks[4], (d,), 0.02),
        'l0_ln1_b': nrm(ks[5], (d,), 0.02),
        'l0_ffn_w_gate': nrm(ks[6], (d, D_FF), d ** -0.5),
        'l0_ffn_w_up': nrm(ks[7], (d, D_FF), d ** -0.5),
        'l0_ffn_w_down': nrm(ks[8], (D_FF, d), D_FF ** -0.5 * DEEPNORM_BETA),
        'l0_ln2_g': 1.0 + nrm(ks[9], (d,), 0.02),
        'l0_ln2_b': nrm(ks[10], (d,), 0.02),
        'l1_attn_w_qkv': jnp.concatenate([nrm(ks[11], (d, 2 * d), d ** -0.5),
                                          nrm(ks[12], (d, d), d ** -0.5 * DEEPNORM_BETA)], axis=1),
        'l1_attn_w_o': nrm(ks[13], (d, d), d ** -0.5 * DEEPNORM_BETA),
        'l1_ln1_g': 1.0 + nrm(ks[14], (d,), 0.02),
        'l1_ln1_b': nrm(ks[15], (d,), 0.02),
        'l1_moe_w_router': nrm(ks[16], (d, N_EXPERTS), d ** -0.5),
        'l1_moe_w_gate': nrm(ks[17], (N_EXPERTS, d, D_FF_EXPERT), d ** -0.5),
        'l1_moe_w_up': nrm(ks[18], (N_EXPERTS, d, D_FF_EXPERT), d ** -0.5),
        'l1_moe_w_down': nrm(ks[19], (N_EXPERTS, D_FF_EXPERT, d), D_FF_EXPERT ** -0.5 * DEEPNORM_BETA),
        'l1_ln2_g': 1.0 + nrm(ks[20], (d,), 0.02),
        'l1_ln2_b': nrm(ks[21], (d,), 0.02),
    }


def reference(x, l0_pool_w_in, l0_pool_w_group, l0_pool_scale, l0_ln1_g, l0_ln1_b,
              l0_ffn_w_gate, l0_ffn_w_up, l0_ffn_w_down, l0_ln2_g, l0_ln2_b,
              l1_attn_w_qkv, l1_attn_w_o, l1_ln1_g, l1_ln1_b,
              l1_moe_w_router, l1_moe_w_gate, l1_moe_w_up, l1_moe_w_down, l1_ln2_g, l1_ln2_b):
    mixers = [
        lambda h: pool_mixer(h, l0_pool_w_in, l0_pool_w_group, l0_pool_scale),
        lambda h: stick_breaking_attention(h, l1_attn_w_qkv, l1_attn_w_o),
    ]
    channel_mixers = [
        lambda h: swiglu(h, l0_ffn_w_gate, l0_ffn_w_up, l0_ffn_w_down),
        lambda h: moe_swiglu(h, l1_moe_w_router, l1_moe_w_gate, l1_moe_w_up, l1_moe_w_down),
    ]
    ln_mix = [(l0_ln1_g, l0_ln1_b), (l1_ln1_g, l1_ln1_b)]
    ln_ffn = [(l0_ln2_g, l0_ln2_b), (l1_ln2_g, l1_ln2_b)]
    for i in range(DEPTH):
        x = layer_norm(DEEPNORM_ALPHA * x + mixers[i % N_MIXERS](x), *ln_mix[i])
        x = layer_norm(DEEPNORM_ALPHA * x + channel_mixers[i](x), *ln_ffn[i])
    return x
```

```python
import math
import os
from contextlib import ExitStack
import numpy as np
import concourse.bass as bass
import concourse.mybir as mybir
from concourse.bass_utils import run_bass_kernel_spmd

F32 = mybir.dt.float32
BF16 = mybir.dt.bfloat16
I32 = mybir.dt.int32
AF = mybir.ActivationFunctionType
ALU = mybir.AluOpType
AX = mybir.AxisListType

NCORES = 8
POOL_WINDOWS = (2, 4, 8, 16)
LN_EPS = 1e-5
HALO = 16

CFG_FULL = dict(S=8192, D=4096, FF=11008, E=8, FFE=5632)


class Counter:
    LIMIT = 30000

    def __init__(self, prog, step):
        self.prog, self.step = prog, step
        self.sems = []
        self.n = 0

    def next_signal(self):
        per = self.LIMIT // self.step
        i = self.n
        self.n += 1
        si = i // per
        while len(self.sems) <= si:
            self.sems.append(self.prog.new_sem())
        return (self.sems[si], (i % per + 1) * self.step)


class Prog:
    ENG = ("pe", "act", "dve", "pool", "sp")

    def __init__(self, nc, stack):
        self.nc, self.stack = nc, stack
        self.nsem = 0
        self.ops = []
        self.last_w = {}
        self.readers = {}
        self.eng_counter = {e: Counter(self, 1) for e in self.ENG}
        self.chan = {}
        self.waited = {e: {} for e in self.ENG}
        self.phase_start = 0
        self.phase_no = 0
        self.cc_keys = {}
        self.last_cc_sig = None
        self.bar = self.new_sem()

    def new_sem(self):
        self.nsem += 1
        sm = self.stack.enter_context(self.nc.semaphore("s%d" % self.nsem))
        if not hasattr(self, "all_sems"):
            self.all_sems = []
        self.all_sems.append(sm)
        return sm

    def op(self, eng, emit, reads=(), writes=(), dma=None, cc=False):
        if cc and os.environ.get("KCC") == "0":
            return None
        i = len(self.ops)
        deps = {}
        for k in reads:
            w = self.last_w.get(k)
            if w is not None:
                deps[w] = "raw"
        for k in writes:
            w = self.last_w.get(k)
            if w is not None and w not in deps:
                deps[w] = "waw"
            for r in self.readers.get(k, ()):
                if r not in deps:
                    deps[r] = "war"
        for k in reads:
            self.readers.setdefault(k, []).append(i)
        for k in writes:
            self.last_w[k] = i
            self.readers[k] = []
        deps.pop(i, None)
        self.ops.append(dict(eng=eng, emit=emit, deps=deps, dma=dma, cc=cc, sig=None, need=False))
        if cc:
            for k in writes:
                self.cc_keys[k] = i
        return i

    stop = None

    def finish(self):
        nc = self.nc
        fin = self.new_sem()
        sems = list(self.all_sems)
        with nc.Block() as block:
            def other(e):
                e.sem_inc(fin, 1)

            def pool(e):
                e.sem_inc(fin, 1)
                e.wait_ge(fin, 5)
                if self.last_cc_sig is not None:
                    e.wait_ge(self.last_cc_sig[0], self.last_cc_sig[1])
                for sm in sems:
                    if sm is not fin:
                        e.sem_clear(sm)
                e.sem_clear(fin)
            block.tensor(other)
            block.scalar(other)
            block.vector(other)
            block.sync(other)
            block.gpsimd(pool)

    def emit_phase(self, block):
        ops = self.ops
        lo = self.phase_start
        if self.stop is not None and self.phase_no >= self.stop:
            self.phase_start = len(ops)
            self.last_w = dict(self.cc_keys)
            self.readers = {}
            return
        for i in range(lo, len(ops)):
            o = ops[i]
            keep = []
            for d, typ in o["deps"].items():
                if d < lo and not ops[d]["cc"]:
                    continue
                y = ops[d]
                y_async = (y["dma"] is not None) or y["cc"]
                x_async = (o["dma"] is not None) or o["cc"]
                if (not y_async) and y["eng"] == o["eng"] and not x_async:
                    if o["eng"] == "pe":
                        continue
                    if typ != "raw":
                        continue
                keep.append(d)
                y["need"] = True
            o["keep"] = keep
        phase_ch = {}
        for i in range(lo, len(ops)):
            o = ops[i]
            if o["dma"] is not None or o["cc"]:
                ch = o["dma"] if o["dma"] is not None else "cc"
                if ch != "cc":
                    if ch not in phase_ch:
                        phase_ch[ch] = "dmach%d" % len(phase_ch)
                    ch = phase_ch[ch]
                o["chname"] = ch
                if ch not in self.chan:
                    self.chan[ch] = Counter(self, 1 if o["cc"] else 16)
                o["sig"] = self.chan[ch].next_signal()
                o["need"] = True
            elif o["need"]:
                o["sig"] = self.eng_counter[o["eng"]].next_signal()
        last_async = {}
        for i in range(lo, len(ops)):
            o = ops[i]
            if o["cc"]:
                self.last_cc_sig = o["sig"]
            elif o["dma"] is not None:
                last_async[(o["eng"], o["chname"])] = o["sig"]

        def run(eng_name, e):
            waited = self.waited[eng_name]
            for i in range(lo, len(ops)):
                o = ops[i]
                if o["eng"] != eng_name:
                    continue
                for d in o["keep"]:
                    sem, val = ops[d]["sig"]
                    key = id(sem)
                    if waited.get(key, 0) >= val:
                        continue
                    e.wait_ge(sem, val)
                    waited[key] = val
                ins = o["emit"](e)
                if o["sig"] is not None:
                    sem, val = o["sig"]
                    step = 16 if (o["dma"] is not None) else 1
                    ins.then_inc(sem, step)
            for (en, ch), (sem, val) in last_async.items():
                if en != eng_name:
                    continue
                key = id(sem)
                if waited.get(key, 0) >= val:
                    continue
                e.wait_ge(sem, val)
                waited[key] = val

        self.phase_no += 1
        pn = self.phase_no

        def run_b(eng_name, e):
            run(eng_name, e)
            if os.environ.get("KBAR") == "0":
                return
            e.drain()
            e.sem_inc(self.bar, 1)
            e.wait_ge(self.bar, 5 * pn)

        block.tensor(lambda e: run_b("pe", e))
        block.scalar(lambda e: run_b("act", e))
        block.vector(lambda e: run_b("dve", e))
        block.gpsimd(lambda e: run_b("pool", e))
        block.sync(lambda e: run_b("sp", e))
        self.phase_start = len(ops)
        self.last_w = dict(self.cc_keys)
        self.readers = {}


def build_program(cfg):
    S, D, FF, E, FFE = cfg["S"], cfg["D"], cfg["FF"], cfg["E"], cfg["FFE"]
    assert E == NCORES
    TOK = S // NCORES
    TT = 256
    NT = TOK // TT
    NB = TT // 128
    KC = D // 128
    NCT = D // 512
    GD = D // 4
    GCH = GD // 128
    NH = D // 128
    HPC = NH // NCORES
    HW_ = HPC * 128
    FC = FF // 128
    FEC = FFE // 128
    DS = D // NCORES
    FS = FF // NCORES
    alpha = float((2 * 2) ** 0.25)

    nc = bass.Bass("TRN2", target_bir_lowering=False)

    def din(name, shape, dt=F32):
        return nc.dram_tensor(name, list(shape), dt, kind="ExternalInput")

    xh = din("xh", [HALO + TOK, D])
    w_in_s = din("w_in_s", [DS, D])
    w_grp_s = din("w_grp_s", [DS, GD])
    vecs = din("vecs", [9, D])
    fg_s = din("fg_s", [DS, FF])
    fu_s = din("fu_s", [DS, FF])
    fd_s = din("fd_s", [FS, D])
    wqkv_c = din("wqkv_c", [D, 3 * HW_])
    wo_s = din("wo_s", [DS, D])
    wr = din("wr", [D, E])
    mg_s = din("mg_s", [D, FFE])
    mu_s = din("mu_s", [D, FFE])
    md_s = din("md_s", [FFE, D])
    consts = din("consts", [128, 4 * 128])
    idx = din("idx", [1, 1], I32)
    coff = din("coff", [128, 1])
    out = nc.dram_tensor("out", [TOK, D], F32, kind="ExternalOutput")

    def dscr(name, shape, dt):
        return nc.dram_tensor(name, list(shape), dt)

    gathered = {}
    shard_specs = [("w_in", w_in_s, DS, D), ("w_grp", w_grp_s, DS, GD), ("fg", fg_s, DS, FF),
                   ("fu", fu_s, DS, FF), ("fd", fd_s, FS, D), ("wo", wo_s, DS, D),
                   ("mg", mg_s, D, FFE), ("mu", mu_s, D, FFE), ("md", md_s, FFE, D)]
    shards = {}
    nsplit = {}
    for nm, src, r, c in shard_specs:
        ns = 1
        while NCORES * (r // ns) * c * 2 > (250 << 20):
            ns *= 2
        assert r % ns == 0 and (ns == 1 or (r // ns) % 128 == 0)
        nsplit[nm] = ns
        shards[nm] = [dscr("%s_b%d" % (nm, h), [r // ns, c], BF16) for h in range(ns)]
        gathered[nm] = [dscr("%s_full%d" % (nm, h), [NCORES * (r // ns), c], BF16) for h in range(ns)]
    def dbg_t(name, shape, dt):
        if cfg.get("debug"):
            return nc.dram_tensor(name, list(shape), dt, kind="ExternalOutput")
        return dscr(name, shape, dt)

    xs1 = dbg_t("xs1", [TOK, D], F32)
    xs2 = dbg_t("xs2", [TOK, D], F32)
    xs3 = dbg_t("xs3", [TOK, D], F32)
    x1T_c = dscr("x1T_c", [D, TOK], BF16)
    x1T_all = dscr("x1T_all", [NCORES * D, TOK], BF16)
    qT = dscr("qT", [HW_, S], BF16)
    kT = dscr("kT", [HW_, S], BF16)
    vv = dscr("vv", [HPC, 128, S // 128, 128], BF16)
    oT_c = dscr("oT_c", [HW_, S], BF16)
    oT_all = dscr("oT_all", [NCORES * HW_, S], BF16)

    gstack = ExitStack()
    with gstack:
        P = Prog(nc, gstack)
        P.stop = cfg.get("stop")

        uid = [0]

        def sb(stack, name, shape, dt):
            uid[0] += 1
            return stack.enter_context(nc.sbuf_tensor("%s_%d" % (name, uid[0]), list(shape), dt))

        def ps(stack, name, shape=(128, 512), dt=F32):
            uid[0] += 1
            return stack.enter_context(nc.psum_tensor("%s_%d" % (name, uid[0]), list(shape), dt))

        ident = sb(gstack, "ident", [128, 128], F32)
        trineg = sb(gstack, "trineg", [128, 128], BF16)
        onesneg = sb(gstack, "onesneg", [128, 128], BF16)
        dmask = sb(gstack, "dmask", [128, 128], BF16)
        coff_t = sb(gstack, "coff_t", [128, 1], F32)

        with nc.Block() as block:
            P.op("sp", lambda e: e.dma_start(out=ident[:], in_=consts[:, 0:128]), writes=["ident"], dma="c_ident")
            P.op("pool", lambda e: e.dma_start(out=trineg[:], in_=consts[:, 128:256]), writes=["trineg"], dma="c_tri")
            P.op("pool", lambda e: e.dma_start(out=onesneg[:], in_=consts[:, 256:384]), writes=["onesneg"], dma="c_ones")
            P.op("pool", lambda e: e.dma_start(out=dmask[:], in_=consts[:, 384:512]), writes=["dmask"], dma="c_dm")
            P.op("sp", lambda e: e.dma_start(out=coff_t[:], in_=coff[:, :]), writes=["coff"], dma="c_coff")
            for nm, src, r, c in shard_specs:
                ns = nsplit[nm]
                rp = r // ns
                rows_per = max(1, min(rp, (4 << 20) // (c * 4)))
                for h in range(ns):
                    r0 = 0
                    pi = 0
                    while r0 < rp:
                        r1 = min(rp, r0 + rows_per)
                        P.op("pool", (lambda e, s=src, d=shards[nm][h], a=r0, b=r1, o=h * rp: e.dma_start(
                            out=d[a:b, :], in_=s[o + a:o + b, :])),
                            writes=[("shard", nm, h, pi)], dma="cast")
                        r0 = r1
                        pi += 1
                    P.op("pool", (lambda e, n=nm, h=h: e.collective_compute(
                        "AllGather", ALU.bypass, replica_groups=[list(range(NCORES))],
                        ins=[shards[n][h].ap().opt()], outs=[gathered[n][h].ap().opt()])),
                        reads=[("shard", nm, h, q) for q in range(pi)], writes=[("full", nm, h)], cc=True)
            P.emit_phase(block)

        def load_xT(stk, src_rows, xT, xblk, tp, evac_eng_cycle, tag, ntok=TT):
            for b in range(ntok // 128):
                P.op("pool", (lambda e, b=b: e.dma_start(out=xblk[:], in_=src_rows(b))), writes=["xblk"], dma=tag + "_xblk")
                for g in range(KC // 4):
                    bank = tp[g % 2]
                    bk = ("tp", g % 2)
                    for q in range(4):
                        k = g * 4 + q
                        P.op("pe", (lambda e, bank=bank, q=q, k=k: e.transpose(
                            out=bank[:, q * 128:(q + 1) * 128], in_=xblk[:, k * 128:(k + 1) * 128], identity=ident[:])),
                            reads=["xblk", "ident"], writes=[bk])
                    eng = evac_eng_cycle[g % len(evac_eng_cycle)]
                    dst = xT[:, g * 4:(g + 1) * 4, b * 128:(b + 1) * 128]
                    srcv = bank[:, 0:512].rearrange("p (q t) -> p q t", q=4)
                    if eng == "act":
                        P.op("act", (lambda e, dst=dst, srcv=srcv: e.activation(out=dst, in_=srcv, func=AF.Copy)),
                             reads=[bk], writes=[("xT", b)])
                    else:
                        P.op("dve", (lambda e, dst=dst, srcv=srcv: e.tensor_copy(out=dst, in_=srcv)),
                             reads=[bk], writes=[("xT", b)])

        def epilogue(Yb, ykey, res_rows, out_rows, g_bc, b_bc, xres, small, tag, xkey="xblk"):
            st, mv, rstd, nmr = small
            P.op("pool", (lambda e: e.dma_start(out=xres[:], in_=res_rows)), writes=[xkey], dma=tag + "_xblk")
            P.op("dve", (lambda e: e.scalar_tensor_tensor(out=Yb, in0=xres[:], scalar=alpha, in1=Yb,
                                                          op0=ALU.mult, op1=ALU.add)),
                 reads=[xkey, ykey], writes=[ykey])
            nchunk = D // 512
            for c in range(nchunk):
                P.op("dve", (lambda e, c=c: e.bn_stats(out=st[:, c * 6:(c + 1) * 6], in_=Yb[:, c * 512:(c + 1) * 512])),
                     reads=[ykey], writes=["st"])
            P.op("dve", (lambda e: e.bn_aggr(out=mv[:], in_=st[:, 0:6 * nchunk].rearrange("p (c s) -> p c s", s=6))),
                 reads=["st"], writes=["mv"])
            P.op("act", (lambda e: e.activation(out=rstd[:], in_=mv[:, 1:2], func=AF.Sqrt, bias=LN_EPS, scale=1.0)),
                 reads=["mv"], writes=["rstd"])
            P.op("dve", (lambda e: e.reciprocal(out=rstd[:], in_=rstd[:])), reads=["rstd"], writes=["rstd"])
            P.op("dve", (lambda e: e.scalar_tensor_tensor(out=nmr[:], in0=mv[:, 0:1], scalar=-1.0, in1=rstd[:],
                                                          op0=ALU.mult, op1=ALU.mult)),
                 reads=["mv", "rstd"], writes=["nmr"])
            P.op("act", (lambda e: e.activation(out=Yb, in_=Yb, func=AF.Identity, bias=nmr[:], scale=rstd[:])),
                 reads=[ykey, "rstd", "nmr"], writes=[ykey])
            P.op("dve", (lambda e: e.tensor_tensor(out=Yb, in0=Yb, in1=g_bc[:], op=ALU.mult)),
                 reads=[ykey, "gbc"], writes=[ykey])
            P.op("dve", (lambda e: e.tensor_tensor(out=Yb, in0=Yb, in1=b_bc[:], op=ALU.add)),
                 reads=[ykey, "bbc"], writes=[ykey])
            P.op("pool", (lambda e: e.dma_start(out=out_rows, in_=Yb)), reads=[ykey], dma=tag + "_st" + str(ykey))

        def load_bc(t, row, key, tag):
            P.op("sp", (lambda e: e.dma_start(out=t[:], in_=vecs[row:row + 1, :].partition_broadcast(128))),
                 writes=[key], dma=tag + "_" + key)

        def small_tiles(stk):
            return (sb(stk, "st", [128, 6 * (D // 512)], F32), sb(stk, "mv", [128, 2], F32),
                    sb(stk, "rstd", [128, 1], F32), sb(stk, "nmr", [128, 1], F32))

        with ExitStack() as stk, nc.Block() as block:
            xT = sb(stk, "xT", [128, KC, TT], BF16)
            xTh = sb(stk, "xTh", [128, KC, HALO], BF16)
            dT = sb(stk, "dT", [128, KC, TT], BF16)
            Y = sb(stk, "Y", [128, NB, D], F32)
            xblk = sb(stk, "xblk", [128, D], F32)
            sc_bc = sb(stk, "sc_bc", [128, D], F32)
            g_bc = sb(stk, "g_bc", [128, D], F32)
            b_bc = sb(stk, "b_bc", [128, D], F32)
            carry = sb(stk, "carry", [128, KC, HALO], F32)
            ub = sb(stk, "ub", [128, HALO + TT], F32)
            sa = sb(stk, "sa", [128, HALO + TT], F32)
            sbb = sb(stk, "sbb", [128, HALO + TT], F32)
            invc = sb(stk, "invc", [128, 4, TT], F32)
            iot = sb(stk, "iot", [128, TT], F32)
            NWB = 3
            wbuf = [sb(stk, "wbuf%d" % i, [128, KC, 256], BF16) for i in range(NWB)]
            gbuf = [sb(stk, "gbuf%d" % i, [128, GCH, 512], BF16) for i in range(2)]
            small = small_tiles(stk)
            tp = [ps(stk, "tp0"), ps(stk, "tp1")]
            up = [ps(stk, "up0"), ps(stk, "up1")]
            yp = [ps(stk, "yp%d" % i) for i in range(4)]
            W = gathered["w_in"][0].ap().rearrange("(k p) n -> p k n", p=128)
            WG = gathered["w_grp"][0].ap().rearrange("(g k p) n -> g p k n", p=128, k=GCH)
            load_bc(sc_bc, 0, "scbc", "p1")
            load_bc(g_bc, 1, "gbc", "p1")
            load_bc(b_bc, 2, "bbc", "p1")
            P.op("pool", (lambda e: e.iota(iot[:], pattern=[[1, TT]], base=0, channel_multiplier=0, allow_small_or_imprecise_dtypes=True)), writes=["iot"])
            wl = [0]

            def load_w(c2):
                slot = wl[0] % NWB
                wl[0] += 1
                P.op("sp", (lambda e, slot=slot, c2=c2: e.dma_start(out=wbuf[slot][:], in_=W[:, :, c2 * 256:(c2 + 1) * 256])),
                     reads=[("full", "w_in", 0)], writes=[("wbuf", slot)], dma="p1_w%d" % slot)
                return slot

            P.op("pool", (lambda e: e.dma_start(out=xblk[0:HALO, :], in_=xh[0:HALO, :])), writes=["xblk"], dma="p1_xblk")
            for g in range(KC // 4):
                bank, bk = tp[g % 2], ("tp", g % 2)
                for q in range(4):
                    k = g * 4 + q
                    P.op("pe", (lambda e, bank=bank, q=q, k=k: e.transpose(
                        out=bank[:, q * HALO:(q + 1) * HALO], in_=xblk[0:HALO, k * 128:(k + 1) * 128],
                        identity=ident[0:HALO, 0:HALO])), reads=["xblk", "ident"], writes=[bk])
                P.op("dve", (lambda e, bank=bank, g=g: e.tensor_copy(
                    out=xTh[:, g * 4:(g + 1) * 4, :], in_=bank[:, 0:4 * HALO].rearrange("p (q t) -> p q t", q=4))),
                    reads=[bk], writes=["xTh"])
            for c2 in range(KC // 2):
                slot = load_w(c2)
                for h in range(2):
                    c = c2 * 2 + h
                    bank, bk = up[c % 2], ("up", c % 2)
                    for k in range(KC):
                        P.op("pe", (lambda e, bank=bank, slot=slot, h=h, k=k: e.matmul(
                            bank[:, 0:HALO], wbuf[slot][:, k, h * 128:(h + 1) * 128], xTh[:, k, :],
                            start=(k == 0), stop=(k == KC - 1))), reads=[("wbuf", slot), "xTh"], writes=[bk])
                    P.op("act", (lambda e, bank=bank, c=c: e.activation(out=carry[:, c, :], in_=bank[:, 0:HALO], func=AF.Copy)),
                         reads=[bk], writes=[("carry", c)])

            for t in range(NT):
                load_xT(stk, (lambda b, t=t: xh[HALO + t * TT + b * 128: HALO + t * TT + (b + 1) * 128, :]),
                        xT, xblk, tp, ["act", "dve"], "p1")
                for g, w in enumerate(POOL_WINDOWS):
                    P.op("dve", (lambda e, g=g, w=w, t=t: e.tensor_scalar(
                        out=invc[:, g, :], in0=iot[:], scalar1=coff_t[:, 0:1], scalar2=float(t * TT + 1),
                        op0=ALU.add, op1=ALU.add)), reads=["iot", "coff"], writes=[("invc", g)])
                    P.op("dve", (lambda e, g=g, w=w: e.tensor_scalar(
                        out=invc[:, g, :], in0=invc[:, g, :], scalar1=float(w), scalar2=None, op0=ALU.min)),
                        reads=[("invc", g)], writes=[("invc", g)])
                    P.op("dve", (lambda e, g=g: e.reciprocal(out=invc[:, g, :], in_=invc[:, g, :])),
                         reads=[("invc", g)], writes=[("invc", g)])
                for c2 in range(KC // 2):
                    slot = load_w(c2)
                    for h in range(2):
                        c = c2 * 2 + h
                        grp = c // GCH
                        bank, bk = up[c % 2], ("up", c % 2)
                        for k in range(KC):
                            P.op("pe", (lambda e, bank=bank, slot=slot, h=h, k=k: e.matmul(
                                bank[:, 0:TT], wbuf[slot][:, k, h * 128:(h + 1) * 128], xT[:, k, :],
                                start=(k == 0), stop=(k == KC - 1))),
                                reads=[("wbuf", slot)] + [("xT", b) for b in range(NB)], writes=[bk])
                        P.op("act", (lambda e, c=c: e.activation(out=ub[:, 0:HALO], in_=carry[:, c, :], func=AF.Copy)),
                             reads=[("carry", c)], writes=["ub"])
                        P.op("act", (lambda e, bank=bank: e.activation(out=ub[:, HALO:HALO + TT], in_=bank[:, 0:TT], func=AF.Copy)),
                             reads=[bk], writes=["ub"])
                        P.op("act", (lambda e, c=c: e.activation(out=carry[:, c, :], in_=ub[:, TT:TT + HALO], func=AF.Copy)),
                             reads=["ub"], writes=[("carry", c)])
                        src, skey = ub, "ub"
                        bufs = [(sa, "sa"), (sbb, "sbb")]
                        sh = 1
                        lo_ = 0
                        for step in range(grp + 1):
                            dst, dkey = bufs[step % 2]
                            lo_ = lo_ + sh
                            P.op("dve", (lambda e, dst=dst, src=src, lo_=lo_, sh=sh: e.tensor_tensor(
                                out=dst[:, lo_:HALO + TT], in0=src[:, lo_:HALO + TT], in1=src[:, lo_ - sh:HALO + TT - sh],
                                op=ALU.add)), reads=[skey], writes=[dkey])
                            src, skey = dst, dkey
                            sh *= 2
                        oth, okey = bufs[(grp + 1) % 2]
                        P.op("dve", (lambda e, oth=oth, src=src, grp=grp: e.tensor_tensor(
                            out=oth[:, HALO:HALO + TT], in0=src[:, HALO:HALO + TT], in1=invc[:, grp, :], op=ALU.mult)),
                            reads=[skey, ("invc", grp)], writes=[okey])
                        P.op("dve", (lambda e, oth=oth, c=c: e.tensor_tensor(
                            out=dT[:, c, :], in0=oth[:, HALO:HALO + TT], in1=ub[:, HALO:HALO + TT], op=ALU.subtract)),
                            reads=[okey, "ub"], writes=[("dT", c)])
                gl = 0
                for n in range(NCT):
                    grp = (n * 512) // GD
                    c0 = (n * 512) % GD
                    gslot = gl % 2
                    gl += 1
                    P.op("sp", (lambda e, gslot=gslot, grp=grp, c0=c0: e.dma_start(out=gbuf[gslot][:], in_=WG[grp][:, :, c0:c0 + 512])),
                         reads=[("full", "w_grp", 0)], writes=[("gbuf", gslot)], dma="p1_g%d" % gslot)
                    for b in range(NB):
                        bank, bk = yp[(n * NB + b) % 4], ("yp", (n * NB + b) % 4)
                        for kk in range(GCH):
                            P.op("pe", (lambda e, bank=bank, gslot=gslot, grp=grp, kk=kk, b=b: e.matmul(
                                bank[:, :], dT[:, grp * GCH + kk, b * 128:(b + 1) * 128], gbuf[gslot][:, kk, :],
                                start=(kk == 0), stop=(kk == GCH - 1))),
                                reads=[("gbuf", gslot)] + [("dT", grp * GCH + kk2) for kk2 in range(GCH)], writes=[bk])
                        P.op("dve", (lambda e, bank=bank, b=b, n=n: e.tensor_tensor(
                            out=Y[:, b, n * 512:(n + 1) * 512], in0=bank[:, :], in1=sc_bc[:, n * 512:(n + 1) * 512], op=ALU.mult)),
                            reads=[bk, "scbc"], writes=[("Y", b)])
                for b in range(NB):
                    r0 = t * TT + b * 128
                    epilogue(Y[:, b, :], ("Y", b), xh[HALO + r0:HALO + r0 + 128, :], xs1[r0:r0 + 128, :],
                             g_bc, b_bc, xblk, small, "p1")
            P.emit_phase(block)

        def kpart(parts, k):
            for i_, (lo_k, hi_k, _) in enumerate(parts):
                if lo_k <= k < hi_k:
                    return i_
            raise AssertionError(k)

        def ffn_phase(tag, src, dst, ln_rows, experts, moe):
            with ExitStack() as stk, nc.Block() as block:
                GJ = 22
                xT = sb(stk, "xT", [128, KC, TT], BF16)
                hT = sb(stk, "hT", [128, GJ, TT], BF16)
                Y = sb(stk, "Y", [128, NB, D], F32)
                xblk = sb(stk, "xblk", [128, D], F32)
                g_bc = sb(stk, "g_bc", [128, D], F32)
                b_bc = sb(stk, "b_bc", [128, D], F32)
                NGU = 2
                gbufs = [sb(stk, "gwb%d" % i, [128, KC, 256], BF16) for i in range(NGU)]
                ubufs = [sb(stk, "uwb%d" % i, [128, KC, 256], BF16) for i in range(NGU)]
                JG = 8
                NDB = 3
                dbufs = [sb(stk, "dwb%d" % i, [128, JG, 512], BF16) for i in range(NDB)]
                sg = sb(stk, "sg", [128, TT], F32)
                small = small_tiles(stk)
                tp = [ps(stk, "tp0"), ps(stk, "tp1")]
                gp = [ps(stk, "gp0"), ps(stk, "gp1")]
                yp = [ps(stk, "yp%d" % i) for i in range(4)]
                if moe:
                    wr_sb = sb(stk, "wr_sb", [128, KC, E], BF16)
                    lg = sb(stk, "lg", [128, NB, E], F32)
                    cw = sb(stk, "cw", [128, NB, E], F32)
                    tm = sb(stk, "tm", [128, 4 * E + 8], F32)
                    P.op("pool", (lambda e: e.dma_start(out=wr_sb[:], in_=wr.ap().rearrange("(k p) n -> p k n", p=128))),
                         writes=["wr"], dma=tag + "_wr")
                load_bc(g_bc, ln_rows[0], "gbc", tag)
                load_bc(b_bc, ln_rows[1], "bbc", tag)
                cnt = dict(gu=0, d=0, first=True)
                for t in range(NT):
                    load_xT(stk, (lambda b, t=t: src[t * TT + b * 128: t * TT + (b + 1) * 128, :]),
                            xT, xblk, tp, ["act", "dve"], tag)
                    xkeys = [("xT", b) for b in range(NB)]
                    if moe:
                        for b in range(NB):
                            bank, bk = tp[b % 2], ("tp", b % 2)
                            for k in range(KC):
                                P.op("pe", (lambda e, bank=bank, b=b, k=k: e.matmul(
                                    bank[:, 0:E], xT[:, k, b * 128:(b + 1) * 128], wr_sb[:, k, :],
                                    start=(k == 0), stop=(k == KC - 1))), reads=["wr", ("xT", b)], writes=[bk])
                            L = lg[:, b, :]
                            m1, m2, ssum = tm[:, 4 * E:4 * E + 1], tm[:, 4 * E + 1:4 * E + 2], tm[:, 4 * E + 2:4 * E + 3]
                            eq, l2, ex = tm[:, 0:E], tm[:, E:2 * E], tm[:, 2 * E:3 * E]
                            kk_ = ("lg", b)
                            P.op("dve", (lambda e, L=L, bank=bank: e.tensor_copy(out=L, in_=bank[:, 0:E])), reads=[bk], writes=[kk_])
                            P.op("dve", (lambda e, L=L, m1=m1: e.tensor_reduce(out=m1, in_=L, axis=AX.X, op=ALU.max)),
                                 reads=[kk_], writes=["tm1"])
                            P.op("dve", (lambda e, L=L, m1=m1, eq=eq: e.tensor_scalar(
                                out=eq, in0=L, scalar1=m1, scalar2=-1e30, op0=ALU.is_ge, op1=ALU.mult)),
                                reads=[kk_, "tm1"], writes=["tm2"])
                            P.op("dve", (lambda e, L=L, eq=eq, l2=l2: e.tensor_tensor(out=l2, in0=L, in1=eq, op=ALU.add)),
                                 reads=[kk_, "tm2"], writes=["tm3"])
                            P.op("dve", (lambda e, l2=l2, m2=m2: e.tensor_reduce(out=m2, in_=l2, axis=AX.X, op=ALU.max)),
                                 reads=["tm3"], writes=["tm4"])
                            P.op("dve", (lambda e, L=L, m2=m2, eq=eq: e.tensor_scalar(
                                out=eq, in0=L, scalar1=m2, scalar2=None, op0=ALU.is_ge)),
                                reads=[kk_, "tm4", "tm3"], writes=["tm5"])
                            P.op("dve", (lambda e, L=L, m1=m1, l2=l2: e.tensor_scalar(
                                out=l2, in0=L, scalar1=m1, scalar2=None, op0=ALU.subtract)),
                                reads=[kk_, "tm1", "tm4"], writes=["tm6"])
                            P.op("act", (lambda e, l2=l2, ex=ex: e.activation(out=ex, in_=l2, func=AF.Exp)),
                                 reads=["tm6"], writes=["tm7"])
                            P.op("dve", (lambda e, ex=ex, eq=eq: e.tensor_tensor(out=ex, in0=ex, in1=eq, op=ALU.mult)),
                                 reads=["tm7", "tm5"], writes=["tm8"])
                            P.op("dve", (lambda e, ex=ex, ssum=ssum: e.tensor_reduce(out=ssum, in_=ex, axis=AX.X, op=ALU.add)),
                                 reads=["tm8"], writes=["tm9"])
                            P.op("dve", (lambda e, ssum=ssum: e.reciprocal(out=ssum, in_=ssum)), reads=["tm9"], writes=["tm10"])
                            P.op("dve", (lambda e, ex=ex, ssum=ssum, b=b: e.tensor_scalar(
                                out=cw[:, b, :], in0=ex, scalar1=ssum, scalar2=None, op0=ALU.mult)),
                                reads=["tm8", "tm10"], writes=[("cw", b)])
                    first = True
                    for ei, (Wg, Wu, Wd, nch, fkeys) in enumerate(experts):
                        j0 = 0
                        while j0 < nch:
                            gj = min(GJ, nch - j0)
                            jj = 0
                            while jj < gj:
                                npair = min(2, gj - jj)
                                slot = cnt["gu"] % NGU
                                cnt["gu"] += 1
                                c0 = (j0 + jj) * 128
                                for hp, (klo, khi, Wap) in enumerate(Wg):
                                    P.op("sp", (lambda e, slot=slot, Wap=Wap, klo=klo, khi=khi, c0=c0, npair=npair: e.dma_start(
                                        out=gbufs[slot][:, klo:khi, 0:npair * 128], in_=Wap[:, :, c0:c0 + npair * 128])),
                                        reads=[("full", fkeys[0], hp)], writes=[("gwb", slot, hp)], dma=tag + "_gw%d_%d" % (slot, hp))
                                for hp, (klo, khi, Wap) in enumerate(Wu):
                                    P.op("sp", (lambda e, slot=slot, Wap=Wap, klo=klo, khi=khi, c0=c0, npair=npair: e.dma_start(
                                        out=ubufs[slot][:, klo:khi, 0:npair * 128], in_=Wap[:, :, c0:c0 + npair * 128])),
                                        reads=[("full", fkeys[1], hp)], writes=[("uwb", slot, hp)], dma=tag + "_uw%d_%d" % (slot, hp))
                                for h in range(npair):
                                    bank, bk = gp[(jj + h) % 2], ("gp", (jj + h) % 2)
                                    for k in range(KC):
                                        P.op("pe", (lambda e, bank=bank, slot=slot, h=h, k=k: e.matmul(
                                            bank[:, 0:TT], gbufs[slot][:, k, h * 128:(h + 1) * 128], xT[:, k, :],
                                            start=(k == 0), stop=(k == KC - 1))),
                                            reads=[("gwb", slot, kpart(Wg, k))] + xkeys, writes=[bk])
                                    for k in range(KC):
                                        P.op("pe", (lambda e, bank=bank, slot=slot, h=h, k=k: e.matmul(
                                            bank[:, TT:2 * TT], ubufs[slot][:, k, h * 128:(h + 1) * 128], xT[:, k, :],
                                            start=(k == 0), stop=(k == KC - 1))),
                                            reads=[("uwb", slot, kpart(Wu, k))] + xkeys, writes=[bk])
                                    P.op("act", (lambda e, bank=bank: e.activation(out=sg[:], in_=bank[:, 0:TT], func=AF.Silu)),
                                         reads=[bk], writes=["sg"])
                                    P.op("dve", (lambda e, bank=bank, jh=jj + h: e.tensor_tensor(
                                        out=hT[:, jh, :], in0=bank[:, TT:2 * TT], in1=sg[:], op=ALU.mult)),
                                        reads=[bk, "sg"], writes=[("hT", jj + h)])
                                jj += npair
                            for n in range(NCT):
                                banks = [(yp[(n % 2) * NB + b], ("yp", (n % 2) * NB + b)) for b in range(NB)]
                                q0 = 0
                                while q0 < gj:
                                    nq = min(JG, gj - q0)
                                    slot = cnt["d"] % NDB
                                    cnt["d"] += 1
                                    dlo, dhi, Dap = Wd[kpart(Wd, j0 + q0)]
                                    assert j0 + q0 + nq <= dhi
                                    P.op("sp", (lambda e, slot=slot, Dap=Dap, r0=j0 + q0 - dlo, nq=nq, n=n: e.dma_start(
                                        out=dbufs[slot][:, 0:nq, :], in_=Dap[:, r0:r0 + nq, n * 512:(n + 1) * 512])),
                                        reads=[("full", fkeys[2], kpart(Wd, j0 + q0))], writes=[("dwb", slot)], dma=tag + "_dw%d" % slot)
                                    for q in range(nq):
                                        for b in range(NB):
                                            bank, bk = banks[b]
                                            P.op("pe", (lambda e, bank=bank, slot=slot, q=q, jq=q0 + q, b=b, gj=gj: e.matmul(
                                                bank[:, :], hT[:, jq, b * 128:(b + 1) * 128], dbufs[slot][:, q, :],
                                                start=(jq == 0), stop=(jq == gj - 1))),
                                                reads=[("dwb", slot), ("hT", q0 + q)], writes=[bk])
                                    q0 += nq
                                for b in range(NB):
                                    bank, bk = banks[b]
                                    Yv = Y[:, b, n * 512:(n + 1) * 512]
                                    yk = ("Y", b)
                                    if moe:
                                        cws = cw[:, b, ei:ei + 1]
                                        if first:
                                            P.op("dve", (lambda e, Yv=Yv, bank=bank, cws=cws: e.tensor_scalar(
                                                out=Yv, in0=bank[:, :], scalar1=cws, scalar2=None, op0=ALU.mult)),
                                                reads=[bk, ("cw", b)], writes=[yk])
                                        else:
                                            P.op("dve", (lambda e, Yv=Yv, bank=bank, cws=cws: e.scalar_tensor_tensor(
                                                out=Yv, in0=bank[:, :], scalar=cws, in1=Yv, op0=ALU.mult, op1=ALU.add)),
                                                reads=[bk, ("cw", b), yk], writes=[yk])
                                    else:
                                        if first:
                                            P.op("act", (lambda e, Yv=Yv, bank=bank: e.activation(out=Yv, in_=bank[:, :], func=AF.Copy)),
                                                 reads=[bk], writes=[yk])
                                        else:
                                            P.op("dve", (lambda e, Yv=Yv, bank=bank: e.tensor_tensor(
                                                out=Yv, in0=bank[:, :], in1=Yv, op=ALU.add)), reads=[bk, yk], writes=[yk])
                            first = False
                            j0 += gj
                    for b in range(NB):
                        r0 = t * TT + b * 128
                        epilogue(Y[:, b, :], ("Y", b), src[r0:r0 + 128, :], dst[r0:r0 + 128, :], g_bc, b_bc, xblk, small, tag)
                P.emit_phase(block)

        Wg = [(0, KC, gathered["fg"][0].ap().rearrange("(k p) n -> p k n", p=128))]
        Wu = [(0, KC, gathered["fu"][0].ap().rearrange("(k p) n -> p k n", p=128))]
        Wd = [(0, FC, gathered["fd"][0].ap().rearrange("(j p) n -> p j n", p=128))]
        ffn_phase("p2", xs1, xs2, (3, 4), [(Wg, Wu, Wd, FC, ("fg", "fu", "fd"))], False)

        with ExitStack() as stk, nc.Block() as block:
            xT = sb(stk, "xT", [128, KC, TT], BF16)
            xblk = sb(stk, "xblk", [128, D], F32)
            tp = [ps(stk, "tp0"), ps(stk, "tp1")]
            x1Tv = x1T_c.ap().rearrange("(k p) t -> p k t", p=128)
            for t in range(NT):
                load_xT(stk, (lambda b, t=t: xs2[t * TT + b * 128: t * TT + (b + 1) * 128, :]), xT, xblk, tp, ["act", "dve"], "p2b")
                P.op("pool", (lambda e, t=t: e.dma_start(out=x1Tv[:, :, t * TT:(t + 1) * TT], in_=xT[:])),
                     reads=[("xT", b) for b in range(NB)], writes=[("x1T", t)], dma="p2b_st")
            P.op("pool", (lambda e: e.collective_compute(
                "AllGather", ALU.bypass, replica_groups=[list(range(NCORES))],
                ins=[x1T_c.ap().opt()], outs=[x1T_all.ap().opt()])),
                reads=[("x1T", t) for t in range(NT)], writes=["x1T_all"], cc=True)
            P.emit_phase(block)

        TQ = min(512, TOK)
        with ExitStack() as stk, nc.Block() as block:
            wq = sb(stk, "wq", [128, KC, 3 * HW_], BF16)
            xTq = [sb(stk, "xTq%d" % i, [128, KC, TQ], BF16) for i in range(2)]
            stg = [sb(stk, "stg%d" % i, [128, 512], BF16) for i in range(4)]
            pp = [ps(stk, "pp%d" % i) for i in range(4)]
            wqv = wqkv_c.ap().rearrange("(k p) n -> p k n", p=128)
            KQ = max(1, KC // 8)
            for i in range(0, KC, KQ):
                P.op("pool", (lambda e, i=i: e.dma_start(out=wq[:, i:i + KQ, :], in_=wqv[:, i:i + KQ, :])),
                     writes=["wq"], dma="p3_wq")
            xall = x1T_all.ap().rearrange("(r k p) t -> r p k t", p=128, k=KC)
            it = 0
            sc = 0
            for r in range(NCORES):
                for s_ in range(TOK // TQ):
                    xs_ = it % 2
                    it += 1
                    tok0 = r * TOK + s_ * TQ
                    P.op("sp", (lambda e, xs_=xs_, r=r, s_=s_: e.dma_start(out=xTq[xs_][:], in_=xall[r][:, :, s_ * TQ:(s_ + 1) * TQ])),
                         reads=["x1T_all"], writes=[("xTq", xs_)], dma="p3_x%d" % xs_)
                    for m in range(2 * HPC):
                        bank, bk = pp[sc % 4], ("pp", sc % 4)
                        sl = sc % 4
                        sc += 1
                        for k in range(KC):
                            P.op("pe", (lambda e, bank=bank, xs_=xs_, m=m, k=k: e.matmul(
                                bank[:, 0:TQ], wq[:, k, m * 128:(m + 1) * 128], xTq[xs_][:, k, :],
                                start=(k == 0), stop=(k == KC - 1))), reads=["wq", ("xTq", xs_)], writes=[bk])
                        scale = (128 ** -0.5) if m < HPC else 1.0
                        P.op("act", (lambda e, bank=bank, sl=sl, scale=scale: e.activation(
                            out=stg[sl][:, 0:TQ], in_=bank[:, 0:TQ], func=AF.Copy, scale=scale)), reads=[bk], writes=[("stg", sl)])
                        dstT = qT if m < HPC else kT
                        hh = m % HPC
                        P.op("pool", (lambda e, dstT=dstT, hh=hh, sl=sl, tok0=tok0: e.dma_start(
                            out=dstT[hh * 128:(hh + 1) * 128, tok0:tok0 + TQ], in_=stg[sl][:, 0:TQ])),
                            reads=[("stg", sl)], dma="p3_st%d" % sl)
                    for b in range(TQ // 128):
                        bank, bk = pp[sc % 4], ("pp", sc % 4)
                        sl = sc % 4
                        sc += 1
                        for k in range(KC):
                            P.op("pe", (lambda e, bank=bank, xs_=xs_, b=b, k=k: e.matmul(
                                bank[:, 0:HW_], xTq[xs_][:, k, b * 128:(b + 1) * 128], wq[:, k, 2 * HW_:3 * HW_],
                                start=(k == 0), stop=(k == KC - 1))), reads=["wq", ("xTq", xs_)], writes=[bk])
                        P.op("dve", (lambda e, bank=bank, sl=sl: e.tensor_copy(out=stg[sl][:, 0:HW_], in_=bank[:, 0:HW_])),
                             reads=[bk], writes=[("stg", sl)])
                        P.op("pool", (lambda e, sl=sl, nb_=(tok0 + b * 128) // 128: e.dma_start(
                            out=vv.ap()[:, :, nb_, :].rearrange("h p d -> p h d"),
                            in_=stg[sl][:, 0:HW_].rearrange("p (h d) -> p h d", h=HPC))),
                             reads=[("stg", sl)], dma="p3_st%d" % sl)
            P.emit_phase(block)

        QC = 512
        NQC = S // QC
        KP4 = int(os.environ.get("KP4", "4"))
        with ExitStack() as stk, nc.Block() as block:
            kTs = [sb(stk, "kTs%d" % i, [128, S], BF16) for i in range(2)]
            qTs = [sb(stk, "qTs%d" % i, [128, S], BF16) for i in range(2)]
            vs = [sb(stk, "vs%d" % i, [128, S // 128, 128], BF16) for i in range(2)]
            E32 = [sb(stk, "E32_%d" % i, [128, QC], F32) for i in range(2)]
            SP = [sb(stk, "SP%d" % i, [128, QC], BF16) for i in range(2)]
            A = [sb(stk, "A%d" % i, [128, QC], BF16) for i in range(2)]
            R = sb(stk, "R", [128, QC], BF16)
            zeros = sb(stk, "zeros", [128, 128], BF16)
            ost = [sb(stk, "ost%d" % i, [128, QC], BF16) for i in range(2)]
            Z = [ps(stk, "Z%d" % i) for i in range(3)]
            Z2 = [ps(stk, "Z2_%d" % i) for i in range(2)]
            O = [ps(stk, "O%d" % i) for i in range(2)]
            if os.environ.get("KMEMSET", "1") == "1":
                P.op("dve", (lambda e: e.memset(zeros[:], 0.0)), writes=["zeros"])
            itn = 0
            for h in range(HPC if KP4 >= 0 else 0):
                hs = h % 2
                P.op("sp", (lambda e, hs=hs, h=h: e.dma_start(out=kTs[hs][:], in_=kT[h * 128:(h + 1) * 128, :])),
                     writes=[("kTs", hs)], dma="p4_k%d" % hs)
                P.op("sp", (lambda e, hs=hs, h=h: e.dma_start(out=qTs[hs][:], in_=qT[h * 128:(h + 1) * 128, :])),
                     writes=[("qTs", hs)], dma="p4_q%d" % hs)
                P.op("sp", (lambda e, hs=hs, h=h: e.dma_start(out=vs[hs][:], in_=vv.ap()[h])),
                     writes=[("vs", hs)], dma="p4_v%d" % hs)
                for i in range(NQC if KP4 >= 1 else 0):
                    Ob, Ok = O[i % 2], ("O", i % 2)
                    os_ = i % 2
                    Q0 = i * QC
                    P.op("pe", (lambda e, Ob=Ob, hs=hs: e.matmul(Ob[:, 0:QC], zeros[:], qTs[hs][:, 0:QC], start=True, stop=False)),
                         reads=["zeros", ("qTs", hs)], writes=[Ok])
                    kb_hi = (Q0 + QC) // 128 - 1
                    for kb in range(kb_hi, -1, -1):
                        zi = itn % 3
                        bi = itn % 2
                        itn += 1
                        Zb, Zk = Z[zi], ("Z", zi)
                        qs = max(0, kb * 128 - Q0)
                        diag = kb * 128 >= Q0
                        firstkb = kb == kb_hi
                        P.op("pe", (lambda e, Zb=Zb, hs=hs, kb=kb, qs=qs, Q0=Q0: e.matmul(
                            Zb[:, qs:QC], kTs[hs][:, kb * 128:(kb + 1) * 128], qTs[hs][:, Q0 + qs:Q0 + QC],
                            start=True, stop=True)), reads=[("kTs", hs), ("qTs", hs)], writes=[Zk])
                        P.op("act", (lambda e, Zb=Zb, bi=bi, qs=qs: e.activation(out=E32[bi][:, qs:QC], in_=Zb[:, qs:QC], func=AF.Exp)),
                             reads=[Zk], writes=[("E32", bi)])
                        P.op("act", (lambda e, bi=bi, qs=qs: e.activation(out=SP[bi][:, qs:QC], in_=E32[bi][:, qs:QC], func=AF.Ln, bias=1.0)),
                             reads=[("E32", bi)], writes=[("SP", bi)])
                        if diag:
                            P.op("dve", (lambda e, bi=bi, qs=qs: e.tensor_tensor(
                                out=SP[bi][:, qs:qs + 128], in0=SP[bi][:, qs:qs + 128], in1=dmask[:], op=ALU.mult)),
                                reads=[("SP", bi), "dmask"], writes=[("SP", bi)])
                        if KP4 < 2:
                            continue
                        Z2b, Z2k = Z2[bi], ("Z2", bi)
                        P.op("pe", (lambda e, Z2b=Z2b, hs=hs, kb=kb, qs=qs, Q0=Q0: e.matmul(
                            Z2b[:, qs:QC], kTs[hs][:, kb * 128:(kb + 1) * 128], qTs[hs][:, Q0 + qs:Q0 + QC],
                            start=True, stop=False)), reads=[("kTs", hs), ("qTs", hs)], writes=[Z2k])
                        P.op("pe", (lambda e, Z2b=Z2b, bi=bi, qs=qs, firstkb=firstkb: e.matmul(
                            Z2b[:, qs:QC], trineg[:], SP[bi][:, qs:QC], start=False, stop=firstkb)),
                            reads=["trineg", ("SP", bi)], writes=[Z2k])
                        if not firstkb:
                            c0_ = qs + 128 if diag else 0
                            P.op("pe", (lambda e, Z2b=Z2b, c0_=c0_: e.matmul(
                                Z2b[:, c0_:QC], onesneg[:], R[:, c0_:QC], start=False, stop=True)),
                                reads=["onesneg", "R"], writes=[Z2k])
                        P.op("act", (lambda e, Z2b=Z2b, bi=bi, qs=qs: e.activation(out=A[bi][:, qs:QC], in_=Z2b[:, qs:QC], func=AF.Exp)),
                             reads=[Z2k], writes=[("A", bi)])
                        if diag:
                            P.op("dve", (lambda e, bi=bi, qs=qs: e.tensor_tensor(
                                out=A[bi][:, qs:qs + 128], in0=A[bi][:, qs:qs + 128], in1=dmask[:], op=ALU.mult)),
                                reads=[("A", bi), "dmask"], writes=[("A", bi)])
                        if KP4 < 3:
                            continue
                        if kb > 0:
                            if firstkb:
                                P.op("dve", (lambda e, bi=bi, qs=qs: e.tensor_copy(out=R[:, qs:QC], in_=SP[bi][:, qs:QC])),
                                     reads=[("SP", bi)], writes=["R"])
                            elif diag:
                                P.op("dve", (lambda e, bi=bi, qs=qs: e.tensor_copy(out=R[:, qs:qs + 128], in_=SP[bi][:, qs:qs + 128])),
                                     reads=[("SP", bi)], writes=["R"])
                                P.op("dve", (lambda e, bi=bi, qs=qs: e.tensor_tensor(
                                    out=R[:, qs + 128:QC], in0=R[:, qs + 128:QC], in1=SP[bi][:, qs + 128:QC], op=ALU.add)),
                                    reads=[("SP", bi), "R"], writes=["R"])
                            else:
                                P.op("dve", (lambda e, bi=bi: e.tensor_tensor(
                                    out=R[:, 0:QC], in0=R[:, 0:QC], in1=SP[bi][:, 0:QC], op=ALU.add)),
                                    reads=[("SP", bi), "R"], writes=["R"])
                        P.op("pe", (lambda e, Ob=Ob, hs=hs, kb=kb, bi=bi, qs=qs: e.matmul(
                            Ob[:, qs:QC], vs[hs][:, kb, :], A[bi][:, qs:QC], start=False, stop=(kb == 0))),
                            reads=[("vs", hs), ("A", bi)], writes=[Ok])
                    if KP4 < 3:
                        continue
                    P.op("dve", (lambda e, Ob=Ob, os_=os_: e.tensor_copy(out=ost[os_][:], in_=Ob[:, 0:QC])),
                         reads=[Ok], writes=[("ost", os_)])
                    P.op("pool", (lambda e, os_=os_, h=h, Q0=Q0: e.dma_start(out=oT_c[h * 128:(h + 1) * 128, Q0:Q0 + QC], in_=ost[os_][:])),
                         reads=[("ost", os_)], writes=["oT_c"], dma="p4_st%d" % os_)
            if KP4 >= 4:
              P.op("pool", (lambda e: e.collective_compute(
                "AllGather", ALU.bypass, replica_groups=[list(range(NCORES))],
                ins=[oT_c.ap().opt()], outs=[oT_all.ap().opt()])),
                reads=["oT_c"], writes=["oT_all"], cc=True)
            P.emit_phase(block)

        with ExitStack() as stk, nc.Block() as block:
            oTt = sb(stk, "oTt", [128, KC, TT], BF16)
            Y = sb(stk, "Y", [128, NB, D], F32)
            xblk = sb(stk, "xblk", [128, D], F32)
            g_bc = sb(stk, "g_bc", [128, D], F32)
            b_bc = sb(stk, "b_bc", [128, D], F32)
            wob = [sb(stk, "wob%d" % i, [128, KC, 512], BF16) for i in range(2)]
            small = small_tiles(stk)
            yp = [ps(stk, "yp%d" % i) for i in range(4)]
            load_bc(g_bc, 5, "gbc", "p5")
            load_bc(b_bc, 6, "bbc", "p5")
            Wo = gathered["wo"][0].ap().rearrange("(k p) n -> p k n", p=128)
            oTv = oT_all.ap().rearrange("(k p) t -> p k t", p=128)
            regs = {}

            def emit_oT_load(e, t):
                if "off" not in regs:
                    r = stk.enter_context(e.register("coreoff"))
                    e.reg_load(r, idx[0:1, 0:1])
                    regs["off"] = e.snap(r)
                off = regs["off"]
                return e.dma_start(out=oTt[:], in_=oTv[:, :, bass.ds(off * TOK + t * TT, TT)])

            wl = 0
            for t in range(NT):
                P.op("sp", (lambda e, t=t: emit_oT_load(e, t)), reads=["oT_all"], writes=["oTt"], dma="p5_oT")
                for n in range(NCT):
                    slot = wl % 2
                    wl += 1
                    P.op("sp", (lambda e, slot=slot, n=n: e.dma_start(out=wob[slot][:], in_=Wo[:, :, n * 512:(n + 1) * 512])),
                         reads=[("full", "wo", 0)], writes=[("wob", slot)], dma="p5_w%d" % slot)
                    for b in range(NB):
                        bank, bk = yp[(n % 2) * NB + b], ("yp", (n % 2) * NB + b)
                        for k in range(KC):
                            P.op("pe", (lambda e, bank=bank, slot=slot, b=b, k=k: e.matmul(
                                bank[:, :], oTt[:, k, b * 128:(b + 1) * 128], wob[slot][:, k, :],
                                start=(k == 0), stop=(k == KC - 1))), reads=["oTt", ("wob", slot)], writes=[bk])
                        P.op("act", (lambda e, bank=bank, b=b, n=n: e.activation(
                            out=Y[:, b, n * 512:(n + 1) * 512], in_=bank[:, :], func=AF.Copy)), reads=[bk], writes=[("Y", b)])
                for b in range(NB):
                    r0 = t * TT + b * 128
                    epilogue(Y[:, b, :], ("Y", b), xs2[r0:r0 + 128, :], xs3[r0:r0 + 128, :], g_bc, b_bc, xblk, small, "p5")
            P.emit_phase(block)

        def eparts(nm, nchunks, e_):
            ns = nsplit[nm]
            cp = nchunks // ns
            return [(h * cp, (h + 1) * cp,
                     gathered[nm][h].ap().rearrange("(e k p) n -> e p k n", p=128, k=cp)[e_]) for h in range(ns)]
        experts = [(eparts("mg", KC, e_), eparts("mu", KC, e_), eparts("md", FEC, e_), FEC, ("mg", "mu", "md")) for e_ in range(E)]
        ffn_phase("p6", xs3, out, (7, 8), experts, True)
        P.finish()
    print("KERNEL build: nsem", P.nsem, "ops", len(P.ops), flush=True)
    return nc


def make_in_maps(cfg, inp):
    S, D, FF, E, FFE = cfg["S"], cfg["D"], cfg["FF"], cfg["E"], cfg["FFE"]
    TOK = S // NCORES
    GD = D // 4
    NH = D // 128
    HPC = NH // NCORES
    HW_ = HPC * 128
    x = np.asarray(inp["x"], np.float32).reshape(S, D)
    xpad = np.concatenate([np.zeros((HALO, D), np.float32), x], axis=0)
    vecs = np.stack([np.asarray(inp[k], np.float32) for k in
                     ["l0_pool_scale", "l0_ln1_g", "l0_ln1_b", "l0_ln2_g", "l0_ln2_b",
                      "l1_ln1_g", "l1_ln1_b", "l1_ln2_g", "l1_ln2_b"]], axis=0)
    ii = np.arange(128)
    ident = np.eye(128, dtype=np.float32)
    trineg = -(ii[:, None] >= ii[None, :]).astype(np.float32)
    onesneg = -np.ones((128, 128), np.float32)
    dm = (ii[:, None] < ii[None, :]).astype(np.float32)
    consts = np.concatenate([ident, trineg, onesneg, dm], axis=1)
    w_grp2 = np.asarray(inp["l0_pool_w_group"], np.float32).reshape(4 * GD, GD)
    wqkv = np.asarray(inp["l1_attn_w_qkv"], np.float32)
    maps = []
    for c in range(NCORES):
        rs = lambda a, n: np.ascontiguousarray(np.asarray(a, np.float32)[c * n:(c + 1) * n])
        cols = np.concatenate([np.arange(j * D + c * HW_, j * D + (c + 1) * HW_) for j in range(3)])
        maps.append({
            "xh": np.ascontiguousarray(xpad[c * TOK: c * TOK + HALO + TOK]),
            "w_in_s": rs(inp["l0_pool_w_in"], D // NCORES),
            "w_grp_s": rs(w_grp2, D // NCORES),
            "vecs": vecs,
            "fg_s": rs(inp["l0_ffn_w_gate"], D // NCORES),
            "fu_s": rs(inp["l0_ffn_w_up"], D // NCORES),
            "fd_s": rs(inp["l0_ffn_w_down"], FF // NCORES),
            "wqkv_c": np.ascontiguousarray(wqkv[:, cols]),
            "wo_s": rs(inp["l1_attn_w_o"], D // NCORES),
            "wr": np.asarray(inp["l1_moe_w_router"], np.float32),
            "mg_s": np.ascontiguousarray(np.asarray(inp["l1_moe_w_gate"], np.float32)[c]),
            "mu_s": np.ascontiguousarray(np.asarray(inp["l1_moe_w_up"], np.float32)[c]),
            "md_s": np.ascontiguousarray(np.asarray(inp["l1_moe_w_down"], np.float32)[c]),
            "consts": consts,
            "idx": np.array([[c]], np.int32),
            "coff": np.full((128, 1), float(c * TOK), np.float32),
        })
    return maps


def run_cfg(cfg, inp):
    nc = build_program(cfg)
    maps = make_in_maps(cfg, inp)
    res = run_bass_kernel_spmd(nc, maps, core_ids=list(range(NCORES)))
    outs = [np.asarray(res.results[c]["out"], np.float32) for c in range(NCORES)]
    full = np.concatenate(outs, axis=0).reshape(1, cfg["S"], cfg["D"])
    if cfg.get("debug"):
        dbg = {k: np.concatenate([np.asarray(res.results[c][k], np.float32) for c in range(NCORES)], axis=0)
               for k in ("xs1", "xs2", "xs3")}
        return full, dbg
    return full


def kernel(**inputs):
    return run_cfg(CFG_FULL, inputs)
```
